# Optimizing a Trainium2 kernel written in Bass

```python
import jax, jax.numpy as jnp
from jax import lax
import numpy as np

D_MODEL = 2048
BATCH = 4
SEQ = 8192
DEPTH = 2

N_MIXERS = 2
N_LAYERS_A = (DEPTH + 1) // 2
N_LAYERS_B = DEPTH // 2
MLSTM_HEADS = 8
QK_HEAD_DIM = D_MODEL // MLSTM_HEADS // 2
V_HEAD_DIM = D_MODEL // MLSTM_HEADS
QK_WIDTH = MLSTM_HEADS * QK_HEAD_DIM
V_WIDTH = MLSTM_HEADS * V_HEAD_DIM
A_IN_WIDTH = 2 * QK_WIDTH + 2 * V_WIDTH + 2 * MLSTM_HEADS
MLSTM_CHUNK = 64
GATE_SOFTCAP = 15.0
CONV_WIDTH = 3
N_EXPERTS = 32
TOP_K = 4
D_EXPERT = D_MODEL
SWIGLU_ALPHA = 1.702
SWIGLU_LIMIT = 7.0
MOE_BLOCK = 512
PLE_DIM = 256
LN_EPS = 1e-5
RMS_EPS = 1e-6
DEEPNORM_ALPHA = (2 * DEPTH) ** 0.25
DEEPNORM_BETA = (8 * DEPTH) ** -0.25

kernel_name = "hybrid_mlstm_shortconv_moe_deepnorm"


def layer_norm(x, g, b):
    xf = x.astype(jnp.float32)
    mu = jnp.mean(xf, axis=-1, keepdims=True)
    xc = xf - mu
    var = jnp.mean(xc * xc, axis=-1, keepdims=True)
    y = xc * lax.rsqrt(var + LN_EPS) * g.astype(jnp.float32) + b.astype(jnp.float32)
    return y.astype(x.dtype)


def mlstm_cell(q, k, v, li, lf):
    bsz, nh, s, dk = q.shape
    dv = v.shape[-1]
    L = MLSTM_CHUNK
    nc = s // L

    def chunks(t):
        return jnp.moveaxis(t.reshape(bsz, nh, nc, L, *t.shape[3:]), 2, 0)

    causal = jnp.tril(jnp.ones((L, L), dtype=bool))

    def step(carry, inp):
        C, n, m = carry
        qc, kc, vc, lic, lfc = inp
        b = jnp.cumsum(lfc, axis=-1)
        dlog = b[..., :, None] - b[..., None, :] + lic[..., None, :]
        dlog = jnp.where(causal, dlog, -jnp.inf)
        inter_log = b + m[..., None]
        m_t = jnp.maximum(jnp.max(dlog, axis=-1), inter_log)
        s_w = jnp.einsum('bhtd,bhsd->bhts', qc, kc) * jnp.exp(dlog - m_t[..., None])
        inter = jnp.exp(inter_log - m_t)
        num = jnp.einsum('bhts,bhsv->bhtv', s_w, vc) + inter[..., None] * jnp.einsum('bhtd,bhdv->bhtv', qc, C)
        den = jnp.sum(s_w, axis=-1) + inter * jnp.einsum('bhtd,bhd->bht', qc, n)
        h = num / jnp.maximum(jnp.abs(den), jnp.exp(-m_t))[..., None]
        b_last = b[..., -1]
        wlog = b_last[..., None] - b + lic
        m_new = jnp.maximum(b_last + m, jnp.max(wlog, axis=-1))
        wk = jnp.exp(wlog - m_new[..., None])
        decay = jnp.exp(b_last + m - m_new)
        C_new = decay[..., None, None] * C + jnp.einsum('bhs,bhsd,bhsv->bhdv', wk, kc, vc)
        n_new = decay[..., None] * n + jnp.einsum('bhs,bhsd->bhd', wk, kc)
        return (C_new, n_new, m_new), h

    init = (jnp.zeros((bsz, nh, dk, dv), jnp.float32),
            jnp.zeros((bsz, nh, dk), jnp.float32),
            jnp.zeros((bsz, nh), jnp.float32))
    _, h = lax.scan(step, init, (chunks(q), chunks(k), chunks(v), chunks(li), chunks(lf)))
    return jnp.moveaxis(h, 0, 2).reshape(bsz, nh, s, dv)


def mlstm_mixer(x, w_in, b_gate, norm_g, w_out):
    bsz, s, _ = x.shape
    proj = x @ w_in
    q, k, v, o, g = jnp.split(proj, [QK_WIDTH, 2 * QK_WIDTH, 2 * QK_WIDTH + V_WIDTH,
                                     2 * QK_WIDTH + 2 * V_WIDTH], axis=-1)
    g = (g + b_gate).astype(jnp.float32)
    g = GATE_SOFTCAP * jnp.tanh(g / GATE_SOFTCAP)
    li = jnp.transpose(g[..., :MLSTM_HEADS], (0, 2, 1))
    lf = jnp.transpose(jax.nn.log_sigmoid(g[..., MLSTM_HEADS:]), (0, 2, 1))

    def heads(t, d):
        return jnp.transpose(t.reshape(bsz, s, MLSTM_HEADS, d), (0, 2, 1, 3)).astype(jnp.float32)

    h = mlstm_cell(heads(q, QK_HEAD_DIM) * (QK_HEAD_DIM ** -0.5), heads(k, QK_HEAD_DIM),
                   heads(v, V_HEAD_DIM), li, lf)
    h = h * lax.rsqrt(jnp.mean(h * h, axis=-1, keepdims=True) + RMS_EPS)
    h = jnp.transpose(h, (0, 2, 1, 3)).reshape(bsz, s, V_WIDTH) * norm_g.astype(jnp.float32)
    h = (h * jax.nn.sigmoid(o.astype(jnp.float32))).astype(x.dtype)
    return h @ w_out


def short_conv_mixer(x, w_in, conv_w, w_out):
    b_gate, c_gate, u = jnp.split(x @ w_in, 3, axis=-1)
    z = c_gate * u
    z = lax.conv_general_dilated(z, conv_w[:, None, :].astype(z.dtype), window_strides=(1,),
                                 padding=[(CONV_WIDTH - 1, 0)],
                                 dimension_numbers=('NWC', 'WIO', 'NWC'),
                                 feature_group_count=z.shape[-1])
    return (b_gate * z) @ w_out


def moe(x, w_router, b_router, w_gu, b_gu, w_dn, b_dn):
    bsz, s, d = x.shape
    n_tok = bsz * s
    xf = x.reshape(n_tok, d)
    logits = xf.astype(jnp.float32) @ w_router.astype(jnp.float32) + b_router.astype(jnp.float32)
    top_v, top_e = lax.top_k(logits, TOP_K)
    gate = jax.nn.softmax(top_v, axis=-1).astype(x.dtype)
    nk = n_tok * TOP_K
    e_flat = top_e.reshape(nk).astype(jnp.int32)
    tok_flat = jnp.arange(nk, dtype=jnp.int32) // TOP_K
    g_flat = gate.reshape(nk)
    order = jnp.argsort(e_flat, stable=True)
    e_sorted = e_flat[order]
    counts = jnp.bincount(e_flat, length=N_EXPERTS).astype(jnp.int32)
    padded = (counts + MOE_BLOCK - 1) // MOE_BLOCK * MOE_BLOCK
    pad_end = jnp.cumsum(padded)
    pad_start = pad_end - padded
    grp_start = jnp.cumsum(counts) - counts
    dest = pad_start[e_sorted] + jnp.arange(nk, dtype=jnp.int32) - grp_start[e_sorted]
    n_rows = -(-nk // MOE_BLOCK) * MOE_BLOCK + N_EXPERTS * MOE_BLOCK
    n_blocks = n_rows // MOE_BLOCK
    row_tok = jnp.zeros((n_rows,), jnp.int32).at[dest].set(tok_flat[order])
    row_gate = jnp.zeros((n_rows,), x.dtype).at[dest].set(g_flat[order])
    blk_start = jnp.arange(n_blocks, dtype=jnp.int32) * MOE_BLOCK
    blk_e = jnp.clip(jnp.searchsorted(pad_end, blk_start, side='right'), 0, N_EXPERTS - 1)

    def expert_block(y, blk):
        tok, gw, e = blk
        h = xf[tok] @ w_gu[e] + b_gu[e]
        g_ = jnp.minimum(h[:, :D_EXPERT], SWIGLU_LIMIT)
        up = jnp.clip(h[:, D_EXPERT:], -SWIGLU_LIMIT, SWIGLU_LIMIT)
        a = (up + 1) * (g_ * jax.nn.sigmoid(SWIGLU_ALPHA * g_))
        out = a @ w_dn[e] + b_dn[e]
        return y.at[tok].add(out * gw[:, None]), None

    y, _ = lax.scan(expert_block, jnp.zeros_like(xf),
                    (row_tok.reshape(n_blocks, MOE_BLOCK), row_gate.reshape(n_blocks, MOE_BLOCK), blk_e))
    return y.reshape(bsz, s, d)


def setup_inputs(seed: int = 0) -> dict:
    key = jax.random.key(seed)
    ks = jax.random.split(key, 24)
    f32 = jnp.float32

    def nrm(k, shape, scale):
        return jax.random.normal(k, shape, f32) * scale

    D, E, F, H = D_MODEL, N_EXPERTS, D_EXPERT, MLSTM_HEADS
    b_gate_a = jnp.concatenate([nrm(ks[5], (N_LAYERS_A, H), 0.1),
                                3.0 + nrm(ks[6], (N_LAYERS_A, H), 0.5)], axis=-1)
    return {
        "x": nrm(ks[0], (BATCH, SEQ, D), 1.0),
        "p": nrm(ks[1], (DEPTH, BATCH, SEQ, PLE_DIM), 1.0),
        "ln_g": 1.0 + nrm(ks[2], (DEPTH, 2, D), 0.02),
        "ln_b": nrm(ks[3], (DEPTH, 2, D), 0.02),
        "w_in_a": nrm(ks[4], (N_LAYERS_A, D, A_IN_WIDTH), D ** -0.5),
        "b_gate_a": b_gate_a,
        "norm_a": 1.0 + nrm(ks[7], (N_LAYERS_A, V_WIDTH), 0.02),
        "w_out_a": nrm(ks[8], (N_LAYERS_A, V_WIDTH, D), V_WIDTH ** -0.5 * DEEPNORM_BETA),
        "w_in_b": nrm(ks[9], (N_LAYERS_B, D, 3 * D), D ** -0.5),
        "conv_b": nrm(ks[10], (N_LAYERS_B, CONV_WIDTH, D), CONV_WIDTH ** -0.5),
        "w_out_b": nrm(ks[11], (N_LAYERS_B, D, D), D ** -0.5 * DEEPNORM_BETA),
        "w_router": nrm(ks[12], (DEPTH, D, E), D ** -0.5),
        "b_router": nrm(ks[13], (DEPTH, E), 0.01),
        "w_gu": nrm(ks[14], (DEPTH, E, D, 2 * F), D ** -0.5),
        "b_gu": nrm(ks[15], (DEPTH, E, 2 * F), 0.01),
        "w_dn": nrm(ks[16], (DEPTH, E, F, D), F ** -0.5 * DEEPNORM_BETA),
        "b_dn": nrm(ks[17], (DEPTH, E, D), 0.01),
        "w_ple_gate": nrm(ks[18], (DEPTH, D, D), D ** -0.5),
        "w_ple_proj": nrm(ks[19], (DEPTH, PLE_DIM, D), PLE_DIM ** -0.5),
    }


def reference(x, p, ln_g, ln_b, w_in_a, b_gate_a, norm_a, w_out_a, w_in_b, conv_b, w_out_b,
              w_router, b_router, w_gu, b_gu, w_dn, b_dn, w_ple_gate, w_ple_proj):
    for i in range(DEPTH):
        j = i // N_MIXERS
        if i % N_MIXERS == 0:
            mix = mlstm_mixer(x, w_in_a[j], b_gate_a[j], norm_a[j], w_out_a[j])
        else:
            mix = short_conv_mixer(x, w_in_b[j], conv_b[j], w_out_b[j])
        x = layer_norm(DEEPNORM_ALPHA * x + mix, ln_g[i, 0], ln_b[i, 0])
        ffn = moe(x, w_router[i], b_router[i], w_gu[i], b_gu[i], w_dn[i], b_dn[i])
        x = layer_norm(DEEPNORM_ALPHA * x + ffn, ln_g[i, 1], ln_b[i, 1])
        x = x + jax.nn.sigmoid(x @ w_ple_gate[i]) * (p[i] @ w_ple_proj[i])
    return x
```

```python
import contextlib
import numpy as np
import concourse.bass as bass
import concourse.mybir as mybir
from concourse.bass_utils import run_bass_kernel_spmd

F32 = mybir.dt.float32
BF16 = mybir.dt.bfloat16
I32 = mybir.dt.int32
U32 = mybir.dt.uint32
ALU = mybir.AluOpType
AF = mybir.ActivationFunctionType

NCORE = 8
BARRIER = True
D = 2048
KT = D // 128
H = 8
DK = 128
DV = 256
PLE = 256
TOPK = 4
ALPHA = 4.0 ** 0.25
LN_EPS = 1e-5
RMS_EPS = 1e-6
SOFTCAP = 15.0
SW_ALPHA = 1.702
SW_LIMIT = 7.0
A_IN = 2 * 1024 + 2 * 2048 + 16


class StopBuild(Exception):
    pass


class Buf:
    __slots__ = ("w", "r")

    def __init__(self):
        self.w = None
        self.r = {}


class K:
    def __init__(self, nc, stack, n_dma_sems=10):
        self.nc = nc
        self.stack = stack
        self.eng = {"pe": nc.tensor, "act": nc.scalar, "dve": nc.vector, "pool": nc.gpsimd, "sp": nc.sync}
        self.sems = {}
        self.cnt = {}
        self.seen = {e: {} for e in self.eng}
        for e in self.eng:
            self.sems[e] = stack.enter_context(nc.semaphore("s_" + e))
            self.cnt[e] = 0
        self.pe_pending = []
        self.dma_ring = {}
        for q in ("sp", "pool"):
            ring = []
            for i in range(n_dma_sems):
                key = f"d_{q}{i}"
                self.sems[key] = stack.enter_context(nc.semaphore(key))
                self.cnt[key] = 0
                ring.append(key)
            self.dma_ring[q] = [ring, 0]
        self.uid = 0

    def name(self, p):
        self.uid += 1
        return f"{p}{self.uid}"

    def sb(self, shape, dtype, st=None):
        return (st or self.stack).enter_context(self.nc.sbuf_tensor(self.name("sb"), list(shape), dtype))

    def ps(self, shape, dtype=F32):
        return self.stack.enter_context(self.nc.psum_tensor(self.name("ps"), list(shape), dtype))

    def dram(self, shape, dtype):
        return self.nc.dram_tensor(self.name("dr"), list(shape), dtype, kind="Internal").ap()

    def _wait(self, e, deps):
        need = {}
        for d in deps:
            if d is None:
                continue
            k, v = d
            if need.get(k, 0) < v:
                need[k] = v
        for k, v in need.items():
            if e == "pe" and k == "pe":
                continue
            if self.seen[e].get(k, 0) < v:
                self.eng[e].wait_ge(self.sems[k], v)
                self.seen[e][k] = v

    @staticmethod
    def _deps(reads, writes):
        deps = []
        for b in reads:
            deps.append(b.w)
        for b in writes:
            deps.append(b.w)
            deps.extend(b.r.items())
        return deps

    def op(self, e, fn, reads=(), writes=(), inc=True):
        self._wait(e, self._deps(reads, writes))
        ins = fn()
        if inc:
            ins.then_inc(self.sems[e], 1)
            self.cnt[e] += 1
            v = self.cnt[e]
            if e == "pe" and self.pe_pending:
                for b in self.pe_pending:
                    if b.r.get(e, 0) < v:
                        b.r[e] = v
                self.pe_pending = []
        else:
            v = self.cnt[e] + 1
            self.pe_pending.extend(reads)
        for b in reads:
            if b.r.get(e, 0) < v:
                b.r[e] = v
        for b in writes:
            b.w = (e, v)
            b.r = {}
        return ins

    def barrier(self):
        if not BARRIER:
            return
        deps = [(key, v) for key, v in self.cnt.items() if v > 0]
        for e in self.eng:
            self._wait(e, deps)

    def dma(self, q, fn, reads=(), writes=(), inc=16):
        ring, idx = self.dma_ring[q]
        key = ring[idx % len(ring)]
        self.dma_ring[q][1] = idx + 1
        deps = self._deps(reads, writes)
        if self.cnt[key] > 0:
            deps.append((key, self.cnt[key]))
        self._wait(q, deps)
        ins = fn()
        if inc == 16:
            ins.then_inc(self.sems[key], 16)
        else:
            ins.then_inc(self.sems[key])
        self.cnt[key] += inc
        v = self.cnt[key]
        for b in reads:
            b.r[key] = v
        for b in writes:
            b.w = (key, v)
            b.r = {}
        return ins


def build(cfg):
    T1 = cfg["T1"]
    E = cfg["E"]
    F = cfg["F"]
    CAP = cfg["CAP"]
    dbg = cfg.get("dbg", False)
    T2 = 2 * T1
    EL = E // NCORE
    FT = F // 128
    NT = T1 // 128
    ST = min(512, T1)
    NTS = ST // 128
    NCH = T2 // 64
    nc = bass.Bass("TRN2", target_bir_lowering=False)

    def ext_in(name, shape, dt=F32):
        return nc.dram_tensor(name, list(shape), dt, kind="ExternalInput").ap()

    x_own = ext_in("x_own", [T1, D])
    x_pre = ext_in("x_pre", [T1, D])
    p_own = ext_in("p_own", [2, T1, PLE])
    hp_in = ext_in("hp", [128, 2])
    zsel_in = ext_in("zsel", [16, 2])
    noag = cfg.get("noag", False)
    MUL = NCORE if noag else 1
    wgu_sh = ext_in("wgu_sh", [2, MUL * EL * D, 2 * F])
    wdn_sh = ext_in("wdn_sh", [2, MUL * EL * F, D])
    wina_sh = ext_in("wina_sh", [MUL * D // NCORE, 6144])
    wouta_sh = ext_in("wouta_sh", [MUL * D // NCORE, D])
    winb_sh = ext_in("winb_sh", [MUL * D // NCORE, 3 * D])
    woutb_sh = ext_in("woutb_sh", [MUL * D // NCORE, D])
    wpg_sh = ext_in("wpg_sh", [2, MUL * D // NCORE, D])
    wpp_sh = ext_in("wpp_sh", [2, MUL * PLE // NCORE, D])
    wgate_in = ext_in("wgate", [128, KT, 40])
    bgate_in = ext_in("bgate", [40, 1])
    wr_in = ext_in("wr", [2, 128, KT, E])
    br_in = ext_in("br", [2, 1, E])
    lng_in = ext_in("lng", [2, 2, D])
    lnb_in = ext_in("lnb", [2, 2, D])
    norma_in = ext_in("norma", [1, D])
    convT_in = ext_in("convT", [128, 3, KT])
    bguT_in = ext_in("bguT", [2, 128, E, 2 * FT])
    bdn_in = ext_in("bdn", [2, E, D])
    ident_in = ext_in("ident", [128, 128])
    tri_in = ext_in("tri", [128, 128])
    cmask_in = ext_in("cmask", [128, 128])
    iota_in = ext_in("iota", [128, 2, E])
    selh_in = ext_in("selh", [8, H, 128])
    y_out = nc.dram_tensor("y", [T1, D], F32, kind="ExternalOutput").ap()
    dbg_out = {}

    with contextlib.ExitStack() as st:
        k = K(nc, st)
        blk = st.enter_context(nc.Block())
        V, A_, P_, PE, SP = nc.vector, nc.scalar, nc.gpsimd, nc.tensor, nc.sync

        ident_f = k.sb([128, 128], F32); ident_b = k.sb([128, 128], BF16)
        tri = k.sb([128, 128], F32); ones_f = k.sb([128, 128], F32)
        cmask = k.sb([128, 128], F32); iota = k.sb([128, 2, E], F32)
        hp = k.sb([128, 2], F32)
        ones_bf = k.sb([1, 128], BF16)
        bC = Buf()
        k.dma("sp", lambda: SP.dma_start(out=ident_f[:], in_=ident_in), writes=[bC])
        k.dma("sp", lambda: SP.dma_start(out=tri[:], in_=tri_in), writes=[bC])
        k.dma("sp", lambda: SP.dma_start(out=cmask[:], in_=cmask_in), writes=[bC])
        k.dma("sp", lambda: SP.dma_start(out=iota[:], in_=iota_in), writes=[bC])
        k.dma("sp", lambda: SP.dma_start(out=hp[:], in_=hp_in), writes=[bC])
        k.op("dve", lambda: V.tensor_copy(out=ident_b[:], in_=ident_f[:]), reads=[bC], writes=[bC])
        k.op("pool", lambda: P_.memset(ones_f[:], 1.0), writes=[bC])
        k.op("pool", lambda: P_.memset(ones_bf[:], 1.0), writes=[bC])

        pf = [(k.ps([128, 512], F32), Buf()) for _ in range(6)]
        pb = [(k.ps([128, 8, 128], BF16), Buf()) for _ in range(2)]
        pfi = [0]; pbi = [0]

        def getpf():
            pfi[0] += 1
            return pf[pfi[0] % len(pf)]

        def getpb():
            pbi[0] += 1
            return pb[pbi[0] % len(pb)]

        def castgather(src, R, C, pieces=1):
            outs = []
            Rp = R // pieces
            if noag:
                for pi in range(pieces):
                    full = k.dram([NCORE * Rp, C], BF16); bf_ = Buf()
                    for r0 in range(0, NCORE * Rp, 1024):
                        r1 = min(NCORE * Rp, r0 + 1024)
                        for c0 in range(0, C, 2048):
                            c1 = min(C, c0 + 2048)
                            k.dma("pool", lambda r0=r0, r1=r1, c0=c0, c1=c1: P_.dma_start(
                                out=full[r0:r1, c0:c1], in_=src[pi * NCORE * Rp + r0:pi * NCORE * Rp + r1, c0:c1]), writes=[bf_])
                    outs.append((full, bf_))
                return outs
            for pi in range(pieces):
                loc = k.dram([Rp, C], BF16); bl = Buf()
                for r0 in range(0, Rp, 1024):
                    r1 = min(Rp, r0 + 1024)
                    for c0 in range(0, C, 2048):
                        c1 = min(C, c0 + 2048)
                        k.dma("pool", lambda r0=r0, r1=r1, c0=c0, c1=c1: P_.dma_start(
                            out=loc[r0:r1, c0:c1], in_=src[pi * Rp + r0:pi * Rp + r1, c0:c1]), writes=[bl])
                full = k.dram([NCORE * Rp, C], BF16); bf_ = Buf()
                k.dma("pool", lambda: P_.collective_compute(
                    "AllGather", ALU.bypass, replica_groups=[list(range(NCORE))],
                    ins=[loc.tensor.ap().opt()], outs=[full.tensor.ap().opt()]), reads=[bl], writes=[bf_], inc=1)
                k._wait("pool", [bf_.w])
                outs.append((full, bf_))
            return outs

        Wina = castgather(wina_sh, D // NCORE, 6144)[0]
        Wouta = castgather(wouta_sh, D // NCORE, D)[0]
        Wgu = [castgather(wgu_sh[l], EL * D, 2 * F, pieces=EL) for l in range(2)]
        Wdn = [castgather(wdn_sh[l], EL * F, D, pieces=EL) for l in range(2)]
        Wpg = [castgather(wpg_sh[l], D // NCORE, D)[0] for l in range(2)]
        Wpp = [castgather(wpp_sh[l], PLE // NCORE, D)[0] for l in range(2)]
        Winb = castgather(winb_sh, D // NCORE, 3 * D)[0]
        Woutb = castgather(woutb_sh, D // NCORE, D)[0]

        if dbg:
            Xmid = nc.dram_tensor("dbg_xmid", [T1, D], F32, kind="ExternalOutput").ap(); bXmid = Buf()
            X1 = nc.dram_tensor("dbg_x1", [T1, D], F32, kind="ExternalOutput").ap(); bX1 = Buf()
        else:
            Xmid = k.dram([T1, D], F32); bXmid = Buf()
            X1 = k.dram([T1, D], F32); bX1 = Buf()
        Xexp = k.dram([E * CAP + 128, D], BF16); bXexp = Buf()
        Yexp = k.dram([E * CAP + 128, D], F32); bYexp = Buf()
        destAll = k.sb([128, NT, TOPK], I32); gAll = k.sb([128, NT, TOPK], F32); bRoute = Buf()
        zt = k.sb([128, D], BF16); bzt = Buf()
        k.op("pool", lambda: P_.memset(zt[:], 0.0), writes=[bzt])
        for r0 in range(0, E * CAP + 128, 128):
            k.dma("sp", lambda r0=r0: SP.dma_start(out=Xexp[r0:r0 + 128, :], in_=zt[:]), reads=[bzt], writes=[bXexp])

        k.dma("pool", lambda: P_.dma_start(out=Yexp[E * CAP:E * CAP + 128, :], in_=zt[:]), reads=[bzt], writes=[bYexp])
        slab = [(k.sb([128, KT, 512], BF16), Buf()) for _ in range(2)]
        sli = [0]

        def load_slab(W, bW, row0, nk, c0, w):
            sli[0] += 1
            s, bs = slab[sli[0] % 2]
            src = W[row0:row0 + nk * 128, c0:c0 + w].rearrange("(kt p) m -> p kt m", p=128)
            k.dma("sp", lambda: SP.dma_start(out=s[:, 0:nk, 0:w], in_=src), reads=[bW], writes=[bs])
            return s, bs

        def dense_tm(xT, bxT, ntok, W, bW, row0, nk, c0, ncols, consumer):
            for cb in range(0, ncols, 512):
                w = min(512, ncols - cb)
                s, bs = load_slab(W, bW, row0, nk, c0 + cb, w)
                for tt in range(ntok // 128):
                    p, bp = getpf()
                    for kt in range(nk):
                        k.op("pe", lambda kt=kt: PE.matmul(p[:, 0:w], lhsT=xT[:, kt, tt * 128:(tt + 1) * 128],
                                                            rhs=s[:, kt, 0:w], start=(kt == 0), stop=(kt == nk - 1)),
                             reads=[bxT, bs], writes=[bp], inc=(kt == nk - 1))
                    consumer(tt, cb, w, p, bp)

        def dense_fm(xT, bxT, ntok, W, bW, row0, nk, c0, ncols, consumer):
            for cb in range(0, ncols, 512):
                w = min(512, ncols - cb)
                s, bs = load_slab(W, bW, row0, nk, c0 + cb, w)
                for f0 in range(0, w, 128):
                    fw = min(128, w - f0)
                    for t0 in range(0, ntok, 512):
                        tw = min(512, ntok - t0)
                        p, bp = getpf()
                        for kt in range(nk):
                            k.op("pe", lambda kt=kt: PE.matmul(p[0:fw, 0:tw], lhsT=s[:, kt, f0:f0 + fw],
                                                                rhs=xT[:, kt, t0:t0 + tw], start=(kt == 0), stop=(kt == nk - 1)),
                                 reads=[bxT, bs], writes=[bp], inc=(kt == nk - 1))
                        consumer((cb + f0) // 128, t0, tw, p, bp)

        def transpose_to(src, bsrc, ncol_tiles, dstT, bdst, tok0, ntok=128):
            for g0 in range(0, ncol_tiles, 8):
                g = min(8, ncol_tiles - g0)
                p, bp = getpb()
                for j in range(g):
                    k.op("pe", lambda j=j: PE.transpose(out=p[:, j, 0:ntok], in_=src[0:ntok, (g0 + j) * 128:(g0 + j + 1) * 128],
                                                        identity=ident_b[0:ntok, 0:ntok]),
                         reads=[bsrc, bC], writes=[bp], inc=(j == g - 1))
                k.op("act", lambda: A_.copy(out=dstT[:, g0:g0 + g, tok0:tok0 + ntok], in_=p[:, 0:g, 0:ntok]),
                     reads=[bp], writes=[bdst])

        xin = [(k.sb([128, D], F32), Buf()) for _ in range(2)]
        xbf = [(k.sb([128, D], BF16), Buf()) for _ in range(2)]
        xii = [0]

        def load_xT(src_dram, bsrc, tok0, ntok, xT, bxT, col0=0):
            for tt in range(ntok // 128):
                xii[0] += 1
                xi, bxi = xin[xii[0] % 2]; xb, bxb = xbf[xii[0] % 2]
                r0 = tok0 + tt * 128
                k.dma("sp", lambda: SP.dma_start(out=xi[:], in_=src_dram[r0:r0 + 128, :]), reads=[bsrc], writes=[bxi])
                k.op("pool", lambda: P_.tensor_copy(out=xb[:], in_=xi[:]), reads=[bxi], writes=[bxb])
                transpose_to(xb, bxb, KT, xT, bxT, col0 + tt * 128)

        def load_bcast(st_, src_row):
            t = k.sb([128, D], F32, st_); b = Buf()
            k.dma("sp", lambda: SP.dma_start(out=t[:].unsqueeze(1), in_=src_row.partition_broadcast(128)), writes=[b])
            return t, b

        def layernorm_tile(r, br_, g, bg, b_, bb, out, bout, st_tmp):
            stats, bst = st_tmp["stats"]
            for c in range(4):
                k.op("dve", lambda c=c: V.bn_stats(out=stats[:, c, :], in_=r[:, c * 512:(c + 1) * 512]), reads=[br_], writes=[bst])
            mv, bmv = st_tmp["mv"]
            k.op("dve", lambda: V.bn_aggr(out=mv[:, 0:2], in_=stats[:].rearrange("p c s -> p (c s)")), reads=[bst], writes=[bmv])
            k.op("dve", lambda: V.tensor_scalar(out=mv[:, 2:3], in0=mv[:, 1:2], scalar1=LN_EPS, scalar2=None, op0=ALU.add), reads=[bmv], writes=[bmv])
            k.op("act", lambda: A_.activation(out=mv[:, 2:3], in_=mv[:, 2:3], func=AF.Sqrt), reads=[bmv], writes=[bmv])
            k.op("dve", lambda: V.reciprocal(out=mv[:, 2:3], in_=mv[:, 2:3]), reads=[bmv], writes=[bmv])
            k.op("dve", lambda: V.tensor_scalar(out=r[:], in0=r[:], scalar1=mv[:, 0:1], scalar2=mv[:, 2:3],
                                                op0=ALU.subtract, op1=ALU.mult), reads=[br_, bmv], writes=[br_])
            k.op("pool", lambda: P_.tensor_tensor(out=r[:], in0=r[:], in1=g[:], op=ALU.mult), reads=[br_, bg], writes=[br_])
            k.op("dve", lambda: V.tensor_tensor(out=out[:], in0=r[:], in1=b_[:], op=ALU.add), reads=[br_, bb], writes=[bout])

        def moe_and_tail(l, out_dram, bout_dram):
            k.barrier()
            with contextlib.ExitStack() as s2:
                XT = k.sb([128, KT, CAP], BF16, s2); bXT = Buf()
                aT = k.sb([128, FT, CAP], BF16, s2); baT = Buf()
                bgu = k.sb([128, E, 2 * FT], F32, s2); bbgu = Buf()
                k.dma("sp", lambda: SP.dma_start(out=bgu[:], in_=bguT_in[l]), writes=[bbgu])
                xe = [(k.sb([128, D], BF16, s2), Buf()) for _ in range(2)]
                gt = [(k.sb([128, 512], F32, s2), Buf()) for _ in range(2)]
                sg = [(k.sb([128, 512], F32, s2), Buf()) for _ in range(2)]
                bdn_sb = [(k.sb([1, D], F32, s2), Buf()) for _ in range(2)]
                bdn_bf = [(k.sb([1, D], BF16, s2), Buf()) for _ in range(2)]
                cnt = [0]
                for e in range(E):
                    pj, rr = e % EL, e // EL
                    Wg, bWg = Wgu[l][pj]; Wd, bWd = Wdn[l][pj]
                    for tt in range(CAP // 128):
                        cnt[0] += 1
                        xt_, bxt_ = xe[cnt[0] % 2]
                        r0 = e * CAP + tt * 128
                        k.dma("sp", lambda: SP.dma_start(out=xt_[:], in_=Xexp[r0:r0 + 128, :]), reads=[bXexp], writes=[bxt_])
                        transpose_to(xt_, bxt_, KT, XT, bXT, tt * 128)
                    def cons_gate(ft, t0, tw, p, bp, e=e):
                        cnt[0] += 1
                        g_, bg_ = gt[cnt[0] % 2]; s_, bs_ = sg[cnt[0] % 2]
                        k.op("dve", lambda: V.tensor_scalar(out=g_[:, 0:tw], in0=p[:, 0:tw], scalar1=bgu[:, e, ft:ft + 1], scalar2=SW_LIMIT,
                                                            op0=ALU.add, op1=ALU.min), reads=[bp, bbgu], writes=[bg_])
                        k.op("act", lambda: A_.activation(out=s_[:, 0:tw], in_=g_[:, 0:tw], func=AF.Sigmoid, scale=SW_ALPHA), reads=[bg_], writes=[bs_])
                        k.op("pool", lambda: P_.tensor_tensor(out=aT[:, ft, t0:t0 + tw], in0=g_[:, 0:tw], in1=s_[:, 0:tw], op=ALU.mult),
                             reads=[bg_, bs_], writes=[baT])
                    dense_fm(XT, bXT, CAP, Wg, bWg, rr * D, KT, 0, F, cons_gate)

                    def cons_up(ft, t0, tw, p, bp, e=e):
                        cnt[0] += 1
                        g_, bg_ = gt[cnt[0] % 2]
                        k.op("dve", lambda: V.tensor_scalar(out=g_[:, 0:tw], in0=p[:, 0:tw], scalar1=bgu[:, e, FT + ft:FT + ft + 1], scalar2=SW_LIMIT,
                                                            op0=ALU.add, op1=ALU.min), reads=[bp, bbgu], writes=[bg_])
                        k.op("dve", lambda: V.tensor_scalar(out=g_[:, 0:tw], in0=g_[:, 0:tw], scalar1=-SW_LIMIT, scalar2=1.0,
                                                            op0=ALU.max, op1=ALU.add), reads=[bg_], writes=[bg_])
                        k.op("pool", lambda: P_.tensor_tensor(out=aT[:, ft, t0:t0 + tw], in0=aT[:, ft, t0:t0 + tw], in1=g_[:, 0:tw], op=ALU.mult),
                             reads=[bg_, baT], writes=[baT])
                    dense_fm(XT, bXT, CAP, Wg, bWg, rr * D, KT, F, F, cons_up)
                    cnt[0] += 1
                    bs32, bbs32 = bdn_sb[cnt[0] % 2]; bsb, bbsb = bdn_bf[cnt[0] % 2]
                    k.dma("sp", lambda: SP.dma_start(out=bs32[:], in_=bdn_in[l, e:e + 1, :]), writes=[bbs32])
                    k.op("dve", lambda: V.tensor_copy(out=bsb[:], in_=bs32[:]), reads=[bbs32], writes=[bbsb])
                    for cb in range(0, D, 512):
                        s, bs = load_slab(Wd, bWd, rr * F, FT, cb, 512)
                        for tt in range(CAP // 128):
                            p, bp = getpf()
                            for kt in range(FT):
                                k.op("pe", lambda kt=kt: PE.matmul(p[:, :], lhsT=aT[:, kt, tt * 128:(tt + 1) * 128], rhs=s[:, kt, :],
                                                                    start=(kt == 0), stop=False), reads=[baT, bs], writes=[bp], inc=False)
                            k.op("pe", lambda: PE.matmul(p[:, :], lhsT=ones_bf[0:1, :], rhs=bsb[0:1, cb:cb + 512], start=False, stop=True),
                                 reads=[bbsb, bC], writes=[bp])
                            cnt[0] += 1
                            y_, by_ = gt[cnt[0] % 2]
                            k.op("act", lambda: A_.copy(out=y_[:, :], in_=p[:, :]), reads=[bp], writes=[by_])
                            r0 = e * CAP + tt * 128
                            k.dma("sp", lambda: SP.dma_start(out=Yexp[r0:r0 + 128, cb:cb + 512], in_=y_[:, :]), reads=[by_], writes=[bYexp])
            k.barrier()
            with contextlib.ExitStack() as s3:
                g2, bg2 = load_bcast(s3, lng_in[l, 1:2, :]); b2, bb2 = load_bcast(s3, lnb_in[l, 1:2, :])
                stt = {"stats": (k.sb([128, 4, 6], F32, s3), Buf()), "mv": (k.sb([128, 4], F32, s3), Buf())}
                xm = [(k.sb([128, D], F32, s3), Buf()) for _ in range(2)]
                ye = [(k.sb([128, D], F32, s3), Buf()) for _ in range(2)]
                x2 = [(k.sb([128, D], F32, s3), Buf()) for _ in range(2)]
                x2b = [(k.sb([128, D], BF16, s3), Buf()) for _ in range(2)]
                x2T = k.sb([128, KT, 128], BF16, s3); bx2T = Buf()
                pt = [(k.sb([128, PLE], F32, s3), Buf()) for _ in range(2)]
                ptb = [(k.sb([128, PLE], BF16, s3), Buf()) for _ in range(2)]
                pT = k.sb([128, PLE // 128, 128], BF16, s3); bpT = Buf()
                sgt = [(k.sb([128, 512], F32, s3), Buf()) for _ in range(2)]
                wpp_sb = k.sb([128, PLE // 128, D], BF16, s3); bwpp = Buf()
                k.dma("sp", lambda: SP.dma_start(out=wpp_sb[:], in_=Wpp[l][0].rearrange("(kt p) m -> p kt m", p=128)), reads=[Wpp[l][1]], writes=[bwpp])
                for tt in range(NT):
                    xm_, bxm_ = xm[tt % 2]; x2_, bx2_ = x2[tt % 2]; x2b_, bx2b_ = x2b[tt % 2]
                    k.dma("sp", lambda: SP.dma_start(out=xm_[:], in_=Xmid[tt * 128:(tt + 1) * 128, :]), reads=[bXmid], writes=[bxm_])
                    k.op("dve", lambda: V.tensor_scalar(out=xm_[:], in0=xm_[:], scalar1=ALPHA, scalar2=None, op0=ALU.mult), reads=[bxm_], writes=[bxm_])
                    for j in range(TOPK):
                        ye_, bye_ = ye[j % 2]
                        k.dma("pool", lambda j=j: P_.indirect_dma_start(
                            out=ye_[:], out_offset=None, in_=Yexp[:, :],
                            in_offset=bass.IndirectOffsetOnAxis(ap=destAll[:, tt, j:j + 1], axis=0),
                            bounds_check=None), reads=[bYexp, bRoute], writes=[bye_])
                        k.op("dve", lambda j=j: V.scalar_tensor_tensor(out=xm_[:], in0=ye_[:], scalar=gAll[:, tt, j:j + 1], in1=xm_[:],
                                                                      op0=ALU.mult, op1=ALU.add), reads=[bye_, bxm_, bRoute], writes=[bxm_])
                    layernorm_tile(xm_, bxm_, g2, bg2, b2, bb2, x2_, bx2_, stt)
                    k.op("act", lambda: A_.copy(out=x2b_[:], in_=x2_[:]), reads=[bx2_], writes=[bx2b_])
                    transpose_to(x2b_, bx2b_, KT, x2T, bx2T, 0)
                    pt_, bpt_ = pt[tt % 2]; ptb_, bptb_ = ptb[tt % 2]
                    k.dma("sp", lambda: SP.dma_start(out=pt_[:], in_=p_own[l, tt * 128:(tt + 1) * 128, :]), writes=[bpt_])
                    k.op("pool", lambda: P_.tensor_copy(out=ptb_[:], in_=pt_[:]), reads=[bpt_], writes=[bptb_])
                    transpose_to(ptb_, bptb_, PLE // 128, pT, bpT, 0)

                    def cons_ple(t_, cb, w, p, bp):
                        s_, bs_ = sgt[(cb // 512) % 2]
                        k.op("act", lambda: A_.activation(out=s_[:, :], in_=p[:, :], func=AF.Sigmoid), reads=[bp], writes=[bs_])
                        p2, bp2 = getpf()
                        for kt in range(PLE // 128):
                            k.op("pe", lambda kt=kt: PE.matmul(p2[:, :], lhsT=pT[:, kt, :], rhs=wpp_sb[:, kt, cb:cb + 512],
                                                                start=(kt == 0), stop=(kt == PLE // 128 - 1)),
                                 reads=[bpT, bwpp], writes=[bp2], inc=(kt == PLE // 128 - 1))
                        k.op("dve", lambda: V.tensor_tensor(out=s_[:, :], in0=s_[:, :], in1=p2[:, :], op=ALU.mult), reads=[bs_, bp2], writes=[bs_])
                        k.op("pool", lambda: P_.tensor_tensor(out=x2_[:, cb:cb + 512], in0=x2_[:, cb:cb + 512], in1=s_[:, :], op=ALU.add),
                             reads=[bs_, bx2_], writes=[bx2_])
                    dense_tm(x2T, bx2T, 128, Wpg[l][0], Wpg[l][1], 0, KT, 0, D, cons_ple)
                    k.dma("sp", lambda: SP.dma_start(out=out_dram[tt * 128:(tt + 1) * 128, :], in_=x2_[:]), reads=[bx2_], writes=[bout_dram])

        def mix_consumer(tiles):
            def cons(tt_, cb, w, p, bp):
                xr, bxr = tiles[tt_]
                k.op("dve", lambda: V.scalar_tensor_tensor(out=xr[:, cb:cb + w], in0=xr[:, cb:cb + w], scalar=ALPHA, in1=p[:, 0:w],
                                                           op0=ALU.mult, op1=ALU.add), reads=[bxr, bp], writes=[bxr])
            return cons

        def post_mixer_tile(l, tt, xres, bxres, ctx):
            g1, bg1, b1, bb1, stt, wr_sb, bwr, br_sb, bbr, cum, bcum = ctx["c"]
            xo, bxo = ctx["xo"][tt % 2]; xob, bxob = ctx["xob"][tt % 2]
            layernorm_tile(xres, bxres, g1, bg1, b1, bb1, xo, bxo, stt)
            k.dma("sp", lambda: SP.dma_start(out=Xmid[tt * 128:(tt + 1) * 128, :], in_=xo[:]), reads=[bxo], writes=[bXmid])
            k.op("act", lambda: A_.copy(out=xob[:], in_=xo[:]), reads=[bxo], writes=[bxob])
            xoT, bxoT = ctx["xoT"]
            for g0 in range(0, KT, 4):
                p, bp = getpf()
                for j in range(4):
                    k.op("pe", lambda j=j: PE.transpose(out=p[:, j * 128:(j + 1) * 128], in_=xo[:, (g0 + j) * 128:(g0 + j + 1) * 128], identity=ident_f[:]),
                         reads=[bxo, bC], writes=[bp], inc=(j == 3))
                k.op("act", lambda: A_.copy(out=xoT[:, g0:g0 + 4, :], in_=p[:, :].rearrange("p (j t) -> p j t", j=4)), reads=[bp], writes=[bxoT])
            pl, bpl = getpf()
            for kt in range(KT):
                k.op("pe", lambda kt=kt: PE.matmul(pl[:, 0:E], lhsT=xoT[:, kt, :], rhs=wr_sb[:, kt, :], start=(kt == 0), stop=(kt == KT - 1)),
                     reads=[bxoT, bwr], writes=[bpl], inc=(kt == KT - 1))
            sm, bsm = ctx["sm"]
            lg = sm[:, 0:E]
            k.op("dve", lambda: V.tensor_tensor(out=lg, in0=pl[:, 0:E], in1=br_sb[:], op=ALU.add), reads=[bpl, bbr], writes=[bsm])
            mx = sm[:, E:E + 8]
            k.op("dve", lambda: V.max(out=mx, in_=lg), reads=[bsm], writes=[bsm])
            mi, bmi = ctx["mi"]
            k.op("dve", lambda: V.max_index(out=mi[:], in_max=mx, in_values=lg), reads=[bsm], writes=[bmi])
            idf = sm[:, E + 8:E + 16]
            k.op("dve", lambda: V.tensor_copy(out=idf, in_=mi[:]), reads=[bmi], writes=[bsm])
            nm = sm[:, E + 16:E + 17]; ssum = sm[:, E + 17:E + 18]; ex = sm[:, E + 18:E + 22]
            k.op("dve", lambda: V.tensor_scalar(out=nm, in0=mx[:, 0:1], scalar1=-1.0, scalar2=None, op0=ALU.mult), reads=[bsm], writes=[bsm])
            k.op("act", lambda: A_.activation(out=ex, in_=mx[:, 0:TOPK], func=AF.Exp, bias=nm, scale=1.0), reads=[bsm], writes=[bsm])
            k.op("dve", lambda: V.reduce_sum(out=ssum, in_=ex, axis=mybir.AxisListType.X), reads=[bsm], writes=[bsm])
            k.op("dve", lambda: V.reciprocal(out=ssum, in_=ssum), reads=[bsm], writes=[bsm])
            k.op("dve", lambda: V.tensor_scalar(out=gAll[:, tt, :], in0=ex, scalar1=ssum, scalar2=None, op0=ALU.mult), reads=[bsm], writes=[bRoute])
            oh, boh = ctx["oh"]
            for j in range(TOPK):
                k.op("dve", lambda j=j: V.tensor_scalar(out=oh[:, j, :], in0=iota[:, 0, :], scalar1=idf[:, j:j + 1], scalar2=None, op0=ALU.is_equal),
                     reads=[bsm, bC], writes=[boh])
            msk = oh[:, TOPK, :]
            k.op("dve", lambda: V.tensor_tensor(out=msk, in0=oh[:, 0, :], in1=oh[:, 1, :], op=ALU.add), reads=[boh], writes=[boh])
            k.op("dve", lambda: V.tensor_tensor(out=msk, in0=msk, in1=oh[:, 2, :], op=ALU.add), reads=[boh], writes=[boh])
            k.op("dve", lambda: V.tensor_tensor(out=msk, in0=msk, in1=oh[:, 3, :], op=ALU.add), reads=[boh], writes=[boh])
            pp, bpp = getpf()
            k.op("pe", lambda: PE.matmul(pp[:, 0:E], lhsT=tri[:, :], rhs=msk, start=True, stop=True), reads=[boh, bC], writes=[bpp])
            pc, bpc = getpf()
            k.op("pe", lambda: PE.matmul(pc[:, 0:E], lhsT=ones_f[:, :], rhs=msk, start=True, stop=True), reads=[boh, bC], writes=[bpc])
            pos = oh[:, TOPK + 1, :]
            k.op("dve", lambda: V.tensor_tensor(out=pos, in0=pp[:, 0:E], in1=cum[:], op=ALU.add), reads=[bpp, bcum], writes=[boh])
            k.op("dve", lambda: V.tensor_tensor(out=cum[:], in0=cum[:], in1=pc[:, 0:E], op=ALU.add), reads=[bpc, boh], writes=[bcum])
            ovf = oh[:, TOPK + 2, :]
            k.op("dve", lambda: V.tensor_scalar(out=ovf, in0=pos, scalar1=float(CAP), scalar2=float(4 * E * CAP), op0=ALU.is_ge, op1=ALU.mult), reads=[boh], writes=[boh])
            k.op("dve", lambda: V.tensor_tensor(out=pos, in0=pos, in1=ovf, op=ALU.add), reads=[boh], writes=[boh])
            k.op("dve", lambda: V.tensor_tensor(out=pos, in0=pos, in1=iota[:, 1, :], op=ALU.add), reads=[boh, bC], writes=[boh])
            k.op("dve", lambda: V.tensor_scalar(out=pos, in0=pos, scalar1=float(E * CAP), scalar2=None, op0=ALU.min), reads=[boh], writes=[boh])
            df = sm[:, E + 22:E + 26]
            junk = oh[:, TOPK + 2, :]
            for j in range(TOPK):
                k.op("dve", lambda j=j: V.tensor_tensor(out=junk, in0=oh[:, j, :], in1=pos, op=ALU.mult), reads=[boh], writes=[boh])
                k.op("dve", lambda j=j: V.reduce_sum(out=df[:, j:j + 1], in_=junk, axis=mybir.AxisListType.X), reads=[boh], writes=[bsm])
            k.op("dve", lambda: V.tensor_copy(out=destAll[:, tt, :], in_=df), reads=[bsm], writes=[bRoute])
            for j in range(TOPK):
                k.dma("pool", lambda j=j: P_.indirect_dma_start(
                    out=Xexp[:, :], out_offset=bass.IndirectOffsetOnAxis(ap=destAll[:, tt, j:j + 1], axis=0),
                    in_=xob[:], in_offset=None, bounds_check=None), reads=[bxob, bRoute], writes=[bXexp])

        def post_ctx(l, s_):
            g1, bg1 = load_bcast(s_, lng_in[l, 0:1, :]); b1, bb1 = load_bcast(s_, lnb_in[l, 0:1, :])
            stt = {"stats": (k.sb([128, 4, 6], F32, s_), Buf()), "mv": (k.sb([128, 4], F32, s_), Buf())}
            wr_sb = k.sb([128, KT, E], F32, s_); bwr = Buf()
            k.dma("sp", lambda: SP.dma_start(out=wr_sb[:], in_=wr_in[l]), writes=[bwr])
            br_sb = k.sb([128, E], F32, s_); bbr = Buf()
            k.dma("sp", lambda: SP.dma_start(out=br_sb[:].unsqueeze(1), in_=br_in[l].partition_broadcast(128)), writes=[bbr])
            cum = k.sb([128, E], F32, s_); bcum = Buf()
            k.op("pool", lambda: P_.memset(cum[:], 0.0), writes=[bcum])
            return {"c": (g1, bg1, b1, bb1, stt, wr_sb, bwr, br_sb, bbr, cum, bcum),
                    "xo": [(k.sb([128, D], F32, s_), Buf()) for _ in range(2)],
                    "xob": [(k.sb([128, D], BF16, s_), Buf()) for _ in range(2)],
                    "xoT": (k.sb([128, KT, 128], F32, s_), Buf()),
                    "sm": (k.sb([128, E + 32], F32, s_), Buf()),
                    "mi": (k.sb([128, 8], U32, s_), Buf()),
                    "oh": (k.sb([128, TOPK + 3, E], F32, s_), Buf())}

        def layer0():
            kT_d = k.dram([H, 128, T2], BF16); bkT = Buf()
            qT_d = k.dram([H, 128, T1], BF16); bqT = Buf()
            k_d = k.dram([T2, H * DK], BF16); bk = Buf()
            v_d = k.dram([T2, H * DV], BF16); bv = Buf()
            og_d = k.dram([T1, D], F32); bog = Buf()
            colq = k.sb([128, T2 // 128, 40], F32); bcolq = Buf()
            carry = k.sb([8, 2], F32); bcar = Buf()
            sG = st.enter_context(contextlib.ExitStack())
            G = k.sb([40, T2], F32, sG); bG = Buf()
            k.barrier()
            with contextlib.ExitStack() as s1:
                xT = k.sb([128, KT, ST], BF16, s1); bxT = Buf()
                wg32 = k.sb([128, KT, 40], F32, s1); wgb = k.sb([128, KT, 40], BF16, s1); bwg = Buf()
                bgt = k.sb([40, 1], F32, s1)
                k.dma("sp", lambda: SP.dma_start(out=wg32[:], in_=wgate_in), writes=[bwg])
                k.dma("sp", lambda: SP.dma_start(out=bgt[:], in_=bgate_in), writes=[bwg])
                k.op("dve", lambda: V.tensor_copy(out=wgb[:], in_=wg32[:]), reads=[bwg], writes=[bwg])
                na, bna = load_bcast(s1, norma_in[0:1, :])
                ev = [(k.sb([128, 512], BF16, s1), Buf()) for _ in range(3)]
                ev32 = [(k.sb([128, 512], F32, s1), Buf()) for _ in range(2)]
                ei = [0]
                for sti in range(T2 // ST):
                    own = sti * ST >= T1
                    tok0 = sti * ST
                    if own:
                        load_xT(x_own, Buf(), tok0 - T1, ST, xT, bxT)
                    else:
                        load_xT(x_pre, Buf(), tok0, ST, xT, bxT)
                    p, bp = getpf()
                    for kt in range(KT):
                        k.op("pe", lambda kt=kt: PE.matmul(p[0:40, 0:ST], lhsT=wgb[:, kt, :], rhs=xT[:, kt, :], start=(kt == 0), stop=(kt == KT - 1)),
                             reads=[bwg, bxT], writes=[bp], inc=(kt == KT - 1))
                    k.op("act", lambda: A_.activation(out=G[:, tok0:tok0 + ST], in_=p[0:40, 0:ST], func=AF.Identity, bias=bgt[:, 0:1], scale=1.0),
                         reads=[bp, bwg], writes=[bG])
                    def cons_kT(ft, t0, tw, p, bp):
                        ei[0] += 1
                        e_, be_ = ev[ei[0] % 3]
                        k.op("act", lambda: A_.copy(out=e_[:, 0:tw], in_=p[:, 0:tw]), reads=[bp], writes=[be_])
                        k.dma("sp", lambda: SP.dma_start(out=kT_d[ft, :, tok0 + t0:tok0 + t0 + tw], in_=e_[:, 0:tw]), reads=[be_], writes=[bkT])
                    dense_fm(xT, bxT, ST, Wina[0], Wina[1], 0, KT, 1024, 1024, cons_kT)
                    if own:
                        def cons_qT(ft, t0, tw, p, bp):
                            ei[0] += 1
                            e_, be_ = ev[ei[0] % 3]
                            k.op("act", lambda: A_.activation(out=e_[:, 0:tw], in_=p[:, 0:tw], func=AF.Copy, scale=float(DK) ** -0.5), reads=[bp], writes=[be_])
                            k.dma("sp", lambda: SP.dma_start(out=qT_d[ft, :, tok0 - T1 + t0:tok0 - T1 + t0 + tw], in_=e_[:, 0:tw]), reads=[be_], writes=[bqT])
                        dense_fm(xT, bxT, ST, Wina[0], Wina[1], 0, KT, 0, 1024, cons_qT)
                    def cons_kv(tt, cb, w, p, bp):
                        ei[0] += 1
                        e_, be_ = ev[ei[0] % 3]
                        k.op("act", lambda: A_.copy(out=e_[:, 0:w], in_=p[:, 0:w]), reads=[bp], writes=[be_])
                        r0 = tok0 + tt * 128
                        if cb < 1024:
                            k.dma("sp", lambda: SP.dma_start(out=k_d[r0:r0 + 128, cb:cb + w], in_=e_[:, 0:w]), reads=[be_], writes=[bk])
                        else:
                            k.dma("sp", lambda: SP.dma_start(out=v_d[r0:r0 + 128, cb - 1024:cb - 1024 + w], in_=e_[:, 0:w]), reads=[be_], writes=[bv])
                    dense_tm(xT, bxT, ST, Wina[0], Wina[1], 0, KT, 1024, 3072, cons_kv)
                    if own:
                        def cons_o(tt, cb, w, p, bp):
                            ei[0] += 1
                            e_, be_ = ev32[ei[0] % 2]
                            k.op("act", lambda: A_.activation(out=e_[:, 0:w], in_=p[:, 0:w], func=AF.Sigmoid), reads=[bp], writes=[be_])
                            k.op("pool", lambda: P_.tensor_tensor(out=e_[:, 0:w], in0=e_[:, 0:w], in1=na[:, cb:cb + w], op=ALU.mult), reads=[be_, bna], writes=[be_])
                            r0 = tok0 - T1 + tt * 128
                            k.dma("sp", lambda: SP.dma_start(out=og_d[r0:r0 + 128, cb:cb + w], in_=e_[:, 0:w]), reads=[be_], writes=[bog])
                        dense_tm(xT, bxT, ST, Wina[0], Wina[1], 0, KT, 4096, 2048, cons_o)
            if cfg.get("stop") == "a1":
                raise StopBuild()
            SEG = min(2048, T2); CPS = SEG // 64
            bS = Buf()
            k.op("pool", lambda: P_.memset(carry[:], 0.0), writes=[bcar])
            k.barrier()
            with contextlib.ExitStack() as s2:
                LI = k.sb([8, SEG], F32, s2); LF = k.sb([8, SEG], F32, s2); Bc = k.sb([8, SEG], F32, s2)
                Aq = k.sb([8, SEG], F32, s2); GM = k.sb([8, SEG], F32, s2); IR = k.sb([8, SEG], F32, s2)
                WK = k.sb([8, SEG], F32, s2); UR = k.sb([8, SEG], F32, s2); GS = k.sb([8, CPS + 1], F32, s2)
                one8 = k.sb([8, 1], F32, s2)
                bT = Buf()
                k.op("pool", lambda: P_.memset(one8[:], 1.0), writes=[bT])
                for sg in range(T2 // SEG):
                    t0 = sg * SEG
                    k.op("act", lambda: A_.activation(out=LI[:], in_=G[0:8, t0:t0 + SEG], func=AF.Tanh, scale=1.0 / SOFTCAP), reads=[bG], writes=[bT])
                    k.op("dve", lambda: V.tensor_scalar(out=LI[:], in0=LI[:], scalar1=SOFTCAP, scalar2=None, op0=ALU.mult), reads=[bT], writes=[bT])
                    k.op("act", lambda: A_.activation(out=LF[:], in_=G[32:40, t0:t0 + SEG], func=AF.Tanh, scale=1.0 / SOFTCAP), reads=[bG], writes=[bT])
                    k.op("act", lambda: A_.activation(out=LF[:], in_=LF[:], func=AF.Exp, scale=-SOFTCAP), reads=[bT], writes=[bT])
                    k.op("act", lambda: A_.activation(out=LF[:], in_=LF[:], func=AF.Ln, bias=1.0, scale=1.0), reads=[bT], writes=[bT])
                    k.op("dve", lambda: V.tensor_scalar(out=LF[:], in0=LF[:], scalar1=-1.0, scalar2=None, op0=ALU.mult), reads=[bT], writes=[bT])
                    if t0 < T1:
                        npre = min(SEG, T1 - t0)
                        k.op("dve", lambda: V.tensor_scalar(out=LF[:, 0:npre], in0=LF[:, 0:npre], scalar1=hp[0:8, 0:1], scalar2=None, op0=ALU.mult), reads=[bT, bC], writes=[bT])
                        k.op("dve", lambda: V.tensor_scalar(out=LI[:, 0:npre], in0=LI[:, 0:npre], scalar1=hp[0:8, 0:1], scalar2=hp[0:8, 1:2], op0=ALU.mult, op1=ALU.add), reads=[bT, bC], writes=[bT])
                    k.op("dve", lambda: V.tensor_tensor_scan(out=Bc[:], data0=one8[:, 0:1].broadcast_to([8, SEG]), data1=LF[:], initial=carry[:, 0:1], op0=ALU.mult, op1=ALU.add), reads=[bT, bcar], writes=[bT])
                    k.op("dve", lambda: V.tensor_tensor(out=Aq[:], in0=LI[:], in1=Bc[:], op=ALU.subtract), reads=[bT], writes=[bT])
                    k.op("dve", lambda: V.tensor_tensor_scan(out=GM[:], data0=Aq[:], data1=Aq[:], initial=carry[:, 1:2], op0=ALU.max, op1=ALU.max), reads=[bT, bcar], writes=[bT])
                    GMv = GM[:].rearrange("p (c l) -> p c l", l=64)
                    k.op("dve", lambda: V.tensor_copy(out=GS[:, 0:1], in_=carry[:, 1:2]), reads=[bcar], writes=[bT])
                    k.op("dve", lambda: V.tensor_copy(out=GS[:, 1:CPS + 1], in_=GMv[:, :, 63]), reads=[bT], writes=[bT])
                    k.op("dve", lambda: V.tensor_tensor(out=IR[:].rearrange("p (c l) -> p c l", l=64), in0=GS[:, 0:CPS].unsqueeze(2).broadcast_to([8, CPS, 64]), in1=GMv, op=ALU.subtract), reads=[bT], writes=[bT])
                    k.op("act", lambda: A_.activation(out=IR[:], in_=IR[:], func=AF.Exp), reads=[bT], writes=[bT])
                    k.op("dve", lambda: V.tensor_tensor(out=WK[:].rearrange("p (c l) -> p c l", l=64), in0=Aq[:].rearrange("p (c l) -> p c l", l=64), in1=GS[:, 1:CPS + 1].unsqueeze(2).broadcast_to([8, CPS, 64]), op=ALU.subtract), reads=[bT], writes=[bT])
                    k.op("act", lambda: A_.activation(out=WK[:], in_=WK[:], func=AF.Exp), reads=[bT], writes=[bT])
                    k.op("dve", lambda: V.tensor_tensor(out=UR[:], in0=Bc[:], in1=GM[:], op=ALU.add), reads=[bT], writes=[bT])
                    k.op("act", lambda: A_.activation(out=UR[:], in_=UR[:], func=AF.Exp, scale=-1.0), reads=[bT], writes=[bT])
                    k.op("dve", lambda: V.tensor_copy(out=carry[:, 0:1], in_=Bc[:, SEG - 1:SEG]), reads=[bT], writes=[bcar])
                    k.op("dve", lambda: V.tensor_copy(out=carry[:, 1:2], in_=GM[:, SEG - 1:SEG]), reads=[bT], writes=[bcar])
                    for tl in range(SEG // 128):
                        ti = (t0 // 128) + tl
                        p, bp = getpf()
                        for qi, R in enumerate((Aq, WK, UR, GM, IR)):
                            k.op("pe", lambda qi=qi, R=R: PE.transpose(out=p[:, qi * 8:(qi + 1) * 8], in_=R[0:8, tl * 128:(tl + 1) * 128], identity=ident_f[0:8, 0:8]),
                                 reads=[bT, bC], writes=[bp], inc=(qi == 4))
                        k.op("act", lambda ti=ti: A_.copy(out=colq[:, ti, :], in_=p[:, 0:40]), reads=[bp], writes=[bcolq])
            sG.close()
            k.barrier()
            if cfg.get("stop") == "a2":
                raise StopBuild()
            k.barrier()
            with contextlib.ExitStack() as s1:
                Cst = [(k.sb([128, DV + 1], F32, s1), Buf()) for _ in range(H)]
                Cbf = [(k.sb([128, DV + 1], BF16, s1), Buf()) for _ in range(H)]
                for h in range(H):
                    k.op("pool", lambda h=h: P_.memset(Cst[h][0][:], 0.0), writes=[Cst[h][1]])
                    k.op("pool", lambda h=h: P_.memset(Cbf[h][0][:], 0.0), writes=[Cbf[h][1]])
                kTt = [(k.sb([128, H, 128], BF16, s1), Buf()) for _ in range(2)]
                qTt = [(k.sb([128, H, 128], BF16, s1), Buf()) for _ in range(2)]
                ktm = [(k.sb([128, H, DK], BF16, s1), Buf()) for _ in range(2)]
                vtm = [(k.sb([128, H, DV + 1], BF16, s1), Buf()) for _ in range(2)]
                for i in range(2):
                    k.op("pool", lambda i=i: P_.memset(vtm[i][0][:], 1.0), writes=[vtm[i][1]])
                kw = [(k.sb([128, DK], BF16, s1), Buf()) for _ in range(2)]
                ogt = [(k.sb([128, D], F32, s1), Buf()) for _ in range(1)]
                hn = [(k.sb([128, D], BF16, s1), Buf()) for _ in range(2)]
                hnT = k.sb([128, KT, 128], BF16, s1); bhnT = Buf()
                Ib = [(k.sb([128, 128], F32, s1), Buf()) for _ in range(2)]
                Mb = [(k.sb([128, 256], F32, s1), Buf()) for _ in range(2)]
                Gb = [(k.sb([128, 128], F32, s1), Buf()) for _ in range(2)]
                Qi = [(k.sb([128, 256], BF16, s1), Buf()) for _ in range(2)]
                for i in range(2):
                    k.op("pool", lambda i=i: P_.memset(Qi[i][0][:], 0.0), writes=[Qi[i][1]])
                Et = [(k.sb([128, 128], F32, s1), Buf()) for _ in range(2)]
                Wt = [(k.sb([128, 128], BF16, s1), Buf()) for _ in range(2)]
                sc = [(k.sb([128, 8], F32, s1), Buf()) for _ in range(2)]
                junk = k.sb([128, DV], F32, s1); bjunk = Buf()
                xres = [(k.sb([128, D], F32, s1), Buf()) for _ in range(1)]
                ctx = post_ctx(0, s1)
                ci = [0]
                for ti in range(T2 // 128):
                    own = ti * 128 >= T1
                    to = ti * 128 - T1
                    if own and cfg.get("a3") == "pre":
                        break
                    kt_, bkt_ = ktm[ti % 2]; vt_, bvt_ = vtm[ti % 2]
                    k.dma("sp", lambda: SP.dma_start(out=kt_[:], in_=k_d[ti * 128:(ti + 1) * 128, :].rearrange("t (h d) -> t h d", h=H)), reads=[bk], writes=[bkt_])
                    k.dma("sp", lambda: SP.dma_start(out=vt_[:, :, 0:DV], in_=v_d[ti * 128:(ti + 1) * 128, :].rearrange("t (h d) -> t h d", h=H)), reads=[bv], writes=[bvt_])
                    if own:
                        kTt_, bkTt_ = kTt[ti % 2]; qTt_, bqTt_ = qTt[ti % 2]
                        k.dma("sp", lambda: SP.dma_start(out=kTt_[:], in_=kT_d[:, :, ti * 128:(ti + 1) * 128].rearrange("h d t -> d h t")), reads=[bkT], writes=[bkTt_])
                        k.dma("sp", lambda: SP.dma_start(out=qTt_[:], in_=qT_d[:, :, to:to + 128].rearrange("h d t -> d h t")), reads=[bqT], writes=[bqTt_])
                        og_, bog_ = ogt[0]
                        k.dma("sp", lambda: SP.dma_start(out=og_[:], in_=og_d[to:to + 128, :]), reads=[bog], writes=[bog_])
                        hn_, bhn_ = hn[ti % 2]
                    for h in range(H):
                        ci[0] += 1
                        Cs, bCs = Cst[h]; Cb, bCb = Cbf[h]
                        Ib_, bIb_ = Ib[ci[0] % 2]
                        Mb_, bMb_ = Mb[ci[0] % 2]
                        pg, bpg = getpf()
                        k.op("dve", lambda: V.tensor_scalar(out=Mb_[:, 128:256], in0=ones_f[:, :], scalar1=colq[:, ti, 32 + h:33 + h], scalar2=None, op0=ALU.mult), reads=[bcolq, bC], writes=[bMb_])
                        k.op("dve", lambda: V.tensor_scalar(out=Mb_[:, 0:128], in0=ones_f[:, :], scalar1=colq[:, ti, 24 + h:25 + h], scalar2=None, op0=ALU.mult), reads=[bcolq, bC], writes=[bMb_])
                        k.op("pe", lambda: PE.transpose(out=pg[:, 0:128], in_=Mb_[:, 0:128], identity=ident_f[:]), reads=[bMb_, bC], writes=[bpg], inc=False)
                        k.op("pe", lambda: PE.transpose(out=pg[:, 128:256], in_=Mb_[:, 128:256], identity=ident_f[:]), reads=[bMb_, bC], writes=[bpg])
                        k.op("act", lambda: A_.copy(out=Ib_[:], in_=pg[:, 128:256]), reads=[bpg], writes=[bIb_])
                        if own:
                            Gb_, bGb_ = Gb[ci[0] % 2]
                            k.op("act", lambda: A_.copy(out=Gb_[:], in_=pg[:, 0:128]), reads=[bpg], writes=[bGb_])
                        ownc = own
                        if ownc:
                            E_, bE_ = Et[ci[0] % 2]; W_, bW_ = Wt[ci[0] % 2]; Qi_, bQi_ = Qi[ci[0] % 2]
                            k.op("dve", lambda: V.tensor_scalar(out=E_[:], in0=Gb_[:], scalar1=-1.0, scalar2=colq[:, ti, h:h + 1], op0=ALU.mult, op1=ALU.add), reads=[bGb_, bcolq], writes=[bE_])
                            k.op("dve", lambda: V.scalar_tensor_tensor(out=E_[:], in0=E_[:], scalar=0.0, in1=cmask[:], op0=ALU.min, op1=ALU.add), reads=[bE_, bC], writes=[bE_])
                            k.op("act", lambda: A_.activation(out=E_[:], in_=E_[:], func=AF.Exp), reads=[bE_], writes=[bE_])
                            k.op("pool", lambda: P_.tensor_tensor(out=Qi_[:, 0:64], in0=qTt_[:, h, 0:64], in1=Ib_[:, 0:64], op=ALU.mult), reads=[bqTt_, bIb_], writes=[bQi_])
                            k.op("pool", lambda: P_.tensor_tensor(out=Qi_[:, 192:256], in0=qTt_[:, h, 64:128], in1=Ib_[:, 64:128], op=ALU.mult), reads=[bqTt_, bIb_], writes=[bQi_])
                            pS, bpS = getpf()
                            k.op("pe", lambda: PE.matmul(pS[:, 0:128], lhsT=kTt_[:, h, :], rhs=qTt_[:, h, :], start=True, stop=True), reads=[bkTt_, bqTt_], writes=[bpS])
                            k.op("dve", lambda: V.tensor_tensor(out=W_[:], in0=pS[:, 0:128], in1=E_[:], op=ALU.mult), reads=[bpS, bE_], writes=[bW_])
                            pN, bpN = getpf()
                            k.op("pe", lambda: PE.matmul(pN[:, 0:DV + 1], lhsT=W_[:, :], rhs=vt_[:, h, :], start=True, stop=False), reads=[bW_, bvt_], writes=[bpN], inc=False)
                        kw_, bkw_ = kw[ci[0] % 2]
                        k.op("act", lambda: A_.activation(out=kw_[:], in_=kt_[:, h, :], func=AF.Copy, scale=colq[:, ti, 8 + h:9 + h]), reads=[bkt_, bcolq], writes=[bkw_])
                        for half in range(2):
                            lo = half * 64
                            if ownc:
                                k.op("pe", lambda: PE.matmul(pN[:, 0:DV + 1], lhsT=Qi_[:, half * 128:(half + 1) * 128], rhs=Cb[:, :], start=False, stop=(half == 1)),
                                     reads=[bQi_, bCb], writes=[bpN])
                            pU, bpU = getpf()
                            k.op("pe", lambda: PE.matmul(pU[:, 0:DV + 1], lhsT=kw_[lo:lo + 64, :], rhs=vt_[lo:lo + 64, h, :], start=True, stop=True),
                                 reads=[bkw_, bvt_], writes=[bpU])
                            k.op("dve", lambda: V.scalar_tensor_tensor(out=Cs[:], in0=Cs[:], scalar=Ib_[:, lo + 63:lo + 64], in1=pU[:, 0:DV + 1], op0=ALU.mult, op1=ALU.add),
                                 reads=[bCs, bIb_, bpU], writes=[bCs])
                            k.op("act", lambda: A_.copy(out=Cb[:], in_=Cs[:]), reads=[bCs], writes=[bCb])
                        if ownc and cfg.get("exp") != "nostats":
                            s_, bs_ = sc[ci[0] % 2]
                            k.op("act", lambda: A_.activation(out=junk[:], in_=pN[:, 0:DV], func=AF.Square, accum_out=s_[:, 0:1]), reads=[bpN], writes=[bjunk, bs_])
                            k.op("act", lambda: A_.activation(out=s_[:, 1:2], in_=pN[:, DV:DV + 1], func=AF.Abs), reads=[bpN], writes=[bs_])
                            k.op("dve", lambda: V.tensor_tensor(out=s_[:, 1:2], in0=s_[:, 1:2], in1=colq[:, ti, 16 + h:17 + h], op=ALU.max), reads=[bs_, bcolq], writes=[bs_])
                            k.op("dve", lambda: V.tensor_tensor(out=s_[:, 1:2], in0=s_[:, 1:2], in1=s_[:, 1:2], op=ALU.mult), reads=[bs_], writes=[bs_])
                            k.op("dve", lambda: V.tensor_scalar(out=s_[:, 0:1], in0=s_[:, 0:1], scalar1=1.0 / DV, scalar2=None, op0=ALU.mult), reads=[bs_], writes=[bs_])
                            k.op("dve", lambda: V.scalar_tensor_tensor(out=s_[:, 2:3], in0=s_[:, 1:2], scalar=RMS_EPS, in1=s_[:, 0:1], op0=ALU.mult, op1=ALU.add), reads=[bs_], writes=[bs_])
                            k.op("act", lambda: A_.activation(out=s_[:, 2:3], in_=s_[:, 2:3], func=AF.Sqrt), reads=[bs_], writes=[bs_])
                            k.op("dve", lambda: V.reciprocal(out=s_[:, 2:3], in_=s_[:, 2:3]), reads=[bs_], writes=[bs_])
                            k.op("dve", lambda: V.scalar_tensor_tensor(out=hn_[:, h * DV:(h + 1) * DV], in0=pN[:, 0:DV], scalar=s_[:, 2:3], in1=og_[:, h * DV:(h + 1) * DV],
                                                                    op0=ALU.mult, op1=ALU.mult), reads=[bpN, bs_, bog_], writes=[bhn_])
                    if own and cfg.get("a3") != "cell":
                        tt = to // 128
                        transpose_to(hn_, bhn_, KT, hnT, bhnT, 0)
                        xr, bxr = xres[0]
                        k.dma("sp", lambda: SP.dma_start(out=xr[:], in_=x_own[to:to + 128, :]), writes=[bxr])
                        dense_tm(hnT, bhnT, 128, Wouta[0], Wouta[1], 0, KT, 0, D, mix_consumer([(xr, bxr)]))
                        post_mixer_tile(0, tt, xr, bxr, ctx)

        def layer1():
            ST = min(256, T1)
            NTS = ST // 128
            k.barrier()
            with contextlib.ExitStack() as s1:
                cw = k.sb([128, 3, KT], F32, s1); bcw = Buf()
                k.dma("sp", lambda: SP.dma_start(out=cw[:], in_=convT_in), writes=[bcw])
                xT = k.sb([128, KT, ST], BF16, s1); bxT = Buf()
                zT = k.sb([128, KT, ST + 2], F32, s1); bzT = Buf()
                yT = k.sb([128, KT, ST], BF16, s1); byT = Buf()
                cgs = [(k.sb([128, 512], F32, s1), Buf()) for _ in range(2)]
                cv = [(k.sb([128, 512], F32, s1), Buf()) for _ in range(2)]
                xres = [(k.sb([128, D], F32, s1), Buf()) for _ in range(NTS)]
                ctx = post_ctx(1, s1)
                load_xT(X1, bX1, T1 - 128, 128, xT, bxT)
                sZ = s1.enter_context(contextlib.ExitStack())
                ztl = k.sb([128, D], F32, sZ); bztl = Buf()
                ztail_d = k.dram([2, D], F32); bzd = Buf()
                ztall_d = k.dram([2 * NCORE, D], F32); bza = Buf()

                def cons_cg(tt, cb, w, p, bp):
                    k.op("act", lambda: A_.copy(out=ztl[:, cb:cb + w], in_=p[:, 0:w]), reads=[bp], writes=[bztl])
                dense_tm(xT, bxT, 128, Winb[0], Winb[1], 0, KT, D, D, cons_cg)

                def cons_u(tt, cb, w, p, bp):
                    k.op("dve", lambda: V.tensor_tensor(out=ztl[:, cb:cb + w], in0=ztl[:, cb:cb + w], in1=p[:, 0:w], op=ALU.mult), reads=[bp, bztl], writes=[bztl])
                dense_tm(xT, bxT, 128, Winb[0], Winb[1], 0, KT, 2 * D, D, cons_u)
                k.dma("sp", lambda: SP.dma_start(out=ztail_d[:, :], in_=ztl[126:128, :]), reads=[bztl], writes=[bzd])
                if noag:
                    for r_ in range(NCORE):
                        k.dma("sp", lambda r_=r_: SP.dma_start(out=ztall_d[2 * r_:2 * r_ + 2, :], in_=ztail_d[:, :]), reads=[bzd], writes=[bza])
                else:
                    k.dma("pool", lambda: P_.collective_compute("AllGather", ALU.bypass, replica_groups=[list(range(NCORE))],
                                                                ins=[ztail_d.tensor.ap().opt()], outs=[ztall_d.tensor.ap().opt()]), reads=[bzd], writes=[bza], inc=1)
                zall = k.sb([16, D], F32, sZ); zsel = k.sb([16, 2], F32, sZ); bzs = Buf()
                k.dma("sp", lambda: SP.dma_start(out=zall[:], in_=ztall_d[:, :]), reads=[bza], writes=[bzs])
                k.dma("sp", lambda: SP.dma_start(out=zsel[:], in_=zsel_in), writes=[bzs])
                for ct in range(KT):
                    p, bp = getpf()
                    k.op("pe", lambda ct=ct: PE.matmul(p[:, 0:2], lhsT=zall[:, ct * 128:(ct + 1) * 128], rhs=zsel[:, :], start=True, stop=True), reads=[bzs], writes=[bp])
                    k.op("act", lambda ct=ct: A_.copy(out=zT[:, ct, 0:2], in_=p[:, 0:2]), reads=[bp], writes=[bzT])
                sZ.close()
                k.barrier()
                for sti in range(T1 // ST):
                    tok0 = sti * ST
                    load_xT(X1, bX1, tok0, ST, xT, bxT)

                    def cons_c(ft, t0, tw, p, bp):
                        c_, bc_ = cgs[ft % 2]
                        k.op("act", lambda: A_.copy(out=c_[:, 0:tw], in_=p[:, 0:tw]), reads=[bp], writes=[bc_])
                        k.op("pool", lambda: P_.tensor_copy(out=zT[:, ft, 2 + t0:2 + t0 + tw], in_=c_[:, 0:tw]), reads=[bc_], writes=[bzT])
                    dense_fm(xT, bxT, ST, Winb[0], Winb[1], 0, KT, D, D, cons_c)

                    def cons_uu(ft, t0, tw, p, bp):
                        k.op("dve", lambda: V.tensor_tensor(out=zT[:, ft, 2 + t0:2 + t0 + tw], in0=zT[:, ft, 2 + t0:2 + t0 + tw], in1=p[:, 0:tw], op=ALU.mult), reads=[bp, bzT], writes=[bzT])
                    dense_fm(xT, bxT, ST, Winb[0], Winb[1], 0, KT, 2 * D, D, cons_uu)

                    def cons_b(ft, t0, tw, p, bp):
                        v_, bv_ = cv[ft % 2]
                        k.op("dve", lambda: V.tensor_scalar(out=v_[:, 0:tw], in0=zT[:, ft, 2 + t0:2 + t0 + tw], scalar1=cw[:, 2, ft:ft + 1], scalar2=None, op0=ALU.mult), reads=[bzT, bcw], writes=[bv_])
                        k.op("dve", lambda: V.scalar_tensor_tensor(out=v_[:, 0:tw], in0=zT[:, ft, 1 + t0:1 + t0 + tw], scalar=cw[:, 1, ft:ft + 1], in1=v_[:, 0:tw], op0=ALU.mult, op1=ALU.add), reads=[bzT, bcw, bv_], writes=[bv_])
                        k.op("dve", lambda: V.scalar_tensor_tensor(out=v_[:, 0:tw], in0=zT[:, ft, t0:t0 + tw], scalar=cw[:, 0, ft:ft + 1], in1=v_[:, 0:tw], op0=ALU.mult, op1=ALU.add), reads=[bzT, bcw, bv_], writes=[bv_])
                        k.op("dve", lambda: V.tensor_tensor(out=yT[:, ft, t0:t0 + tw], in0=v_[:, 0:tw], in1=p[:, 0:tw], op=ALU.mult), reads=[bv_, bp], writes=[byT])
                    dense_fm(xT, bxT, ST, Winb[0], Winb[1], 0, KT, 0, D, cons_b)
                    k.op("pool", lambda: P_.tensor_copy(out=zT[:, :, 0:2], in_=zT[:, :, ST:ST + 2]), reads=[bzT], writes=[bzT])
                    for t_ in range(NTS):
                        tt = sti * NTS + t_
                        xr, bxr = xres[t_]
                        k.dma("sp", lambda: SP.dma_start(out=xr[:], in_=X1[tt * 128:(tt + 1) * 128, :]), reads=[bX1], writes=[bxr])
                    dense_tm(yT, byT, ST, Woutb[0], Woutb[1], 0, KT, 0, D, mix_consumer(xres))
                    for t_ in range(NTS):
                        post_mixer_tile(1, sti * NTS + t_, xres[t_][0], xres[t_][1], ctx)

        stop = cfg.get("stop")
        bY = Buf()
        try:
            if stop == "w":
                raise StopBuild()
            layer0()
            if stop == "l0":
                raise StopBuild()
            moe_and_tail(0, X1, bX1)
            if stop == "m0":
                raise StopBuild()
            layer1()
            if stop == "l1":
                raise StopBuild()
            moe_and_tail(1, y_out, bY)
        except StopBuild:
            pass
        k._wait("sp", [(key, v) for key, v in k.cnt.items() if v > 0])
        if cfg.get("clear", False):
            fin = st.enter_context(nc.semaphore("fin"))
            nc.sync.sem_inc(fin, 1)
            nc.gpsimd.wait_ge(fin, 1)
            for sem in list(k.sems.values()) + [fin]:
                nc.gpsimd.sem_clear(sem)
    return nc


FULL = dict(T1=4096, E=32, F=2048, CAP=640)


def make_inputs(cfg, x, p, ln_g, ln_b, w_in_a, b_gate_a, norm_a, w_out_a, w_in_b, conv_b, w_out_b,
                w_router, b_router, w_gu, b_gu, w_dn, b_dn, w_ple_gate, w_ple_proj):
    T1, E, F, CAP = cfg["T1"], cfg["E"], cfg["F"], cfg["CAP"]
    f = np.float32
    x = np.asarray(x, f); p = np.asarray(p, f)
    EL = E // NCORE
    FT = F // 128
    wa = np.asarray(w_in_a, f)[0]
    wgate = np.zeros((D, 40), f)
    wgate[:, 0:8] = wa[:, 6144:6152]
    wgate[:, 32:40] = wa[:, 6152:6160]
    wgate = np.ascontiguousarray(wgate.reshape(KT, 128, 40).transpose(1, 0, 2))
    bgate = np.zeros((40, 1), f)
    bgate[0:8, 0] = np.asarray(b_gate_a, f)[0, 0:8]
    bgate[32:40, 0] = np.asarray(b_gate_a, f)[0, 8:16]
    wr = np.ascontiguousarray(np.asarray(w_router, f).reshape(2, KT, 128, E).transpose(0, 2, 1, 3))
    br = np.asarray(b_router, f).reshape(2, 1, E)
    convT = np.ascontiguousarray(np.asarray(conv_b, f)[0].reshape(3, KT, 128).transpose(2, 0, 1))
    bguT = np.ascontiguousarray(np.asarray(b_gu, f).reshape(2, E, 2 * FT, 128).transpose(0, 3, 1, 2))
    ident = np.eye(128, dtype=f)
    tri = np.triu(np.ones((128, 128), f), 1)
    ii = np.arange(128)
    cmask = np.where((ii[:, None] <= ii[None, :]) & ((ii[:, None] // 64) == (ii[None, :] // 64)), 0.0, -30000.0).astype(f)
    iota = np.zeros((128, 2, E), f)
    iota[:, 0, :] = np.arange(E)
    iota[:, 1, :] = np.arange(E) * CAP
    selh = np.zeros((8, H, 128), f)
    for h in range(H):
        selh[h, h, :] = 1.0
    wgu = np.asarray(w_gu, f); wdn = np.asarray(w_dn, f)
    wia = wa[:, 0:6144]
    woa = np.asarray(w_out_a, f)[0]; wib = np.asarray(w_in_b, f)[0]; wob = np.asarray(w_out_b, f)[0]
    wpg = np.asarray(w_ple_gate, f); wpp = np.asarray(w_ple_proj, f)
    S = x.shape[1]
    maps = []
    for c in range(NCORE):
        b, hf = c // 2, c % 2
        sl = slice(hf * T1, (hf + 1) * T1)
        hpv = np.zeros((128, 2), f)
        hpv[:, 0] = float(hf)
        hpv[:, 1] = (float(hf) - 1.0) * 1e4
        zsel = np.zeros((16, 2), f)
        if hf == 1:
            zsel[2 * (c - 1), 0] = 1.0
            zsel[2 * (c - 1) + 1, 1] = 1.0
        rs = slice(c * (D // NCORE), (c + 1) * (D // NCORE))
        m = {
            "x_own": np.ascontiguousarray(x[b, sl]),
            "x_pre": np.ascontiguousarray(x[b, 0:T1]),
            "p_own": np.ascontiguousarray(p[:, b, sl]),
            "hp": hpv, "zsel": zsel,
            "wgu_sh": np.ascontiguousarray(wgu[:, c * EL:(c + 1) * EL].reshape(2, EL * D, 2 * F)),
            "wdn_sh": np.ascontiguousarray(wdn[:, c * EL:(c + 1) * EL].reshape(2, EL * F, D)),
            "wina_sh": np.ascontiguousarray(wia[rs]), "wouta_sh": np.ascontiguousarray(woa[rs]),
            "winb_sh": np.ascontiguousarray(wib[rs]), "woutb_sh": np.ascontiguousarray(wob[rs]),
            "wpg_sh": np.ascontiguousarray(wpg[:, rs]),
            "wpp_sh": np.ascontiguousarray(wpp[:, c * (PLE // NCORE):(c + 1) * (PLE // NCORE)]),
            "wgate": wgate, "bgate": bgate, "wr": wr, "br": br,
            "lng": np.asarray(ln_g, f), "lnb": np.asarray(ln_b, f),
            "norma": np.asarray(norm_a, f).reshape(1, D), "convT": convT, "bguT": bguT,
            "bdn": np.asarray(b_dn, f), "ident": ident, "tri": tri, "cmask": cmask, "iota": iota, "selh": selh,
        }
        maps.append(m)
    return maps


def _noag_maps(cfg, maps):
    E, F = cfg["E"], cfg["F"]
    EL = E // NCORE

    def full(key, pieces, per_layer):
        def one(shards):
            Rp = shards[0].shape[0] // pieces
            return np.concatenate([np.concatenate([sh[pi * Rp:(pi + 1) * Rp] for sh in shards], 0) for pi in range(pieces)], 0)
        if per_layer:
            return np.stack([one([m[key][l] for m in maps]) for l in range(2)], 0)
        return one([m[key] for m in maps])
    rep = {"wgu_sh": full("wgu_sh", EL, True), "wdn_sh": full("wdn_sh", EL, True),
           "wina_sh": full("wina_sh", 1, False), "wouta_sh": full("wouta_sh", 1, False),
           "winb_sh": full("winb_sh", 1, False), "woutb_sh": full("woutb_sh", 1, False),
           "wpg_sh": full("wpg_sh", 1, True), "wpp_sh": full("wpp_sh", 1, True)}
    for m in maps:
        m.update(rep)
    return maps


_NC_CACHE = {}


def run(cfg, **inputs):
    key = tuple(sorted((k_, v) for k_, v in cfg.items()))
    if key not in _NC_CACHE:
        _NC_CACHE[key] = build(cfg)
    nc = _NC_CACHE[key]
    maps = make_inputs({k_: v for k_, v in cfg.items()}, **inputs)
    ncr = cfg.get("ncores", NCORE)
    if cfg.get("noag"):
        maps = _noag_maps(cfg, maps)
    maps = maps[:ncr]
    res = run_bass_kernel_spmd(nc, maps, core_ids=list(range(ncr)))
    if cfg.get("dbg"):
        return res
    T1 = cfg["T1"]
    B = NCORE // 2
    out = np.zeros((B, 2 * T1, D), np.float32)
    for c in range(NCORE):
        out[c // 2, (c % 2) * T1:(c % 2 + 1) * T1] = res.results[c]["y"]
    return out


def kernel(**inputs):
    return run(FULL, **inputs)
```
